# Optimizing a Trainium2 kernel written in Bass

```python
import jax
import jax.numpy as jnp
from jax import lax
import numpy as np

D_MODEL = 2048
BATCH = 4
SEQ = 8192
DEPTH = 1

GRID_W = 64
CTX_LEN = 256
ATTN_HEADS = 16
ATTN_KV_HEADS = 4
HEAD_DIM = 64
GQA_GROUP = ATTN_HEADS // ATTN_KV_HEADS
WINDOW = 128
ATTN_BLOCK = 128
ROPE_BASE = 10000.0
GLA_HEADS = 4
GLA_DK = 128
GLA_DV = 256
GLA_GATE_RANK = 16
GLA_GATE_TEMP = 16.0
GLA_CHUNK = 64
ATTN_WIDTH = ATTN_HEADS * HEAD_DIM
GLA_WIDTH = GLA_HEADS * GLA_DV
MIX_WIDTH = ATTN_WIDTH + GLA_WIDTH
IN_SPLITS = (ATTN_WIDTH, ATTN_KV_HEADS * HEAD_DIM, ATTN_KV_HEADS * HEAD_DIM,
             GLA_HEADS * GLA_DK, GLA_HEADS * GLA_DK, GLA_WIDTH, GLA_WIDTH, 2 * GLA_GATE_RANK)
IN_WIDTH = sum(IN_SPLITS)
N_EXPERTS = 32
TOP_K = 4
D_EXPERT = D_MODEL
SWIGLU_LIMIT = 7.0
SWIGLU_ALPHA = 1.702
MOE_BLOCK = 256
NORM_EPS = 1e-6

kernel_name = 'hybrid_swa_gla_moe_dit_layer'


def rms_norm(x, g):
    xf = x.astype(jnp.float32)
    y = xf * lax.rsqrt(jnp.mean(xf * xf, axis=-1, keepdims=True) + NORM_EPS)
    return (y * g.astype(jnp.float32)).astype(x.dtype)


def modulate(h, shift, scale):
    return h * (1 + scale) + shift


def axial_rope_angles(n):
    rows = n // GRID_W
    row = jnp.repeat(jnp.arange(rows, dtype=jnp.float32), GRID_W)
    col = jnp.tile(jnp.arange(GRID_W, dtype=jnp.float32), rows)
    n_freq = HEAD_DIM // 4
    inv_freq = ROPE_BASE ** (-jnp.arange(n_freq, dtype=jnp.float32) / n_freq)
    ang = jnp.stack([row[:, None] * inv_freq, col[:, None] * inv_freq], axis=1)
    return jnp.cos(ang), jnp.sin(ang)


def apply_axial_rope(x, cos, sin):
    b, n, h, d = x.shape
    xf = x.astype(jnp.float32).reshape(b, n, h, 2, 2, d // 4)
    x1, x2 = xf[..., 0, :], xf[..., 1, :]
    c, s = cos[None, :, None], sin[None, :, None]
    out = jnp.stack([x1 * c - x2 * s, x1 * s + x2 * c], axis=-2)
    return out.reshape(b, n, h, d).astype(x.dtype)


def split_projection(p):
    b, l, _ = p.shape
    q_a, k_a, v_a, q_g, k_g, v_g, r_g, z_g = jnp.split(p, np.cumsum(IN_SPLITS)[:-1].tolist(), axis=-1)
    return (q_a.reshape(b, l, ATTN_HEADS, HEAD_DIM),
            k_a.reshape(b, l, ATTN_KV_HEADS, HEAD_DIM),
            v_a.reshape(b, l, ATTN_KV_HEADS, HEAD_DIM),
            q_g.reshape(b, l, GLA_HEADS, GLA_DK),
            k_g.reshape(b, l, GLA_HEADS, GLA_DK),
            v_g.reshape(b, l, GLA_HEADS, GLA_DV),
            r_g,
            z_g.reshape(b, l, 2, GLA_GATE_RANK))


def window_attention(q, k, v, k_ctx, v_ctx, sink):
    b, n = q.shape[:2]
    m = k_ctx.shape[1]
    n_blocks = n // ATTN_BLOCK
    band = ATTN_BLOCK + 2 * WINDOW
    qg = (q * HEAD_DIM ** -0.5).reshape(b, n, ATTN_KV_HEADS, GQA_GROUP, HEAD_DIM)
    pad = ((0, 0), (WINDOW, WINDOW), (0, 0), (0, 0))
    k_pad = jnp.pad(k, pad)
    v_pad = jnp.pad(v, pad)
    offset = jnp.arange(band)[None, :] - jnp.arange(ATTN_BLOCK)[:, None]
    in_window = (offset >= 0) & (offset <= 2 * WINDOW)
    sink_col = jnp.broadcast_to(sink.astype(jnp.float32).reshape(1, ATTN_KV_HEADS, GQA_GROUP, 1, 1),
                                (b, ATTN_KV_HEADS, GQA_GROUP, ATTN_BLOCK, 1))

    def block(bi):
        start = bi * ATTN_BLOCK
        qb = lax.dynamic_slice_in_dim(qg, start, ATTN_BLOCK, axis=1)
        kb = lax.dynamic_slice_in_dim(k_pad, start, band, axis=1)
        vb = lax.dynamic_slice_in_dim(v_pad, start, band, axis=1)
        key_pos = start - WINDOW + jnp.arange(band)
        valid = in_window & ((key_pos >= 0) & (key_pos < n))[None, :]
        s_loc = jnp.einsum('bqkgd,bskd->bkgqs', qb, kb).astype(jnp.float32)
        s_loc = jnp.where(valid, s_loc, -jnp.inf)
        s_ctx = jnp.einsum('bqkgd,bskd->bkgqs', qb, k_ctx).astype(jnp.float32)
        p = jax.nn.softmax(jnp.concatenate([s_loc, s_ctx, sink_col], axis=-1), axis=-1).astype(v.dtype)
        o = (jnp.einsum('bkgqs,bskd->bqkgd', p[..., :band], vb)
             + jnp.einsum('bkgqs,bskd->bqkgd', p[..., band:band + m], v_ctx))
        return o.reshape(b, ATTN_BLOCK, ATTN_WIDTH)

    out = lax.map(block, jnp.arange(n_blocks))
    return jnp.moveaxis(out, 0, 1).reshape(b, n, ATTN_WIDTH)


def context_attention(q_ctx, k_ctx, v_ctx, sink):
    b, m = q_ctx.shape[:2]
    qg = (q_ctx * HEAD_DIM ** -0.5).reshape(b, m, ATTN_KV_HEADS, GQA_GROUP, HEAD_DIM)
    s = jnp.einsum('bqkgd,bskd->bkgqs', qg, k_ctx).astype(jnp.float32)
    sink_col = jnp.broadcast_to(sink.astype(jnp.float32).reshape(1, ATTN_KV_HEADS, GQA_GROUP, 1, 1),
                                (b, ATTN_KV_HEADS, GQA_GROUP, m, 1))
    p = jax.nn.softmax(jnp.concatenate([s, sink_col], axis=-1), axis=-1)[..., :m].astype(v_ctx.dtype)
    o = jnp.einsum('bkgqs,bskd->bqkgd', p, v_ctx)
    return o.reshape(b, m, ATTN_WIDTH)


def gla_log_gate(z, w_a2, b_a2):
    b, l = z.shape[:2]
    g = jax.nn.log_sigmoid((z @ w_a2 + b_a2).astype(jnp.float32)) / GLA_GATE_TEMP
    return g.reshape(b, l, GLA_HEADS, GLA_DK)


def gla_scan(q, k, v, log_a, state0):
    b, l, h, _ = q.shape
    nc = l // GLA_CHUNK

    def to_chunks(t):
        return jnp.moveaxis(t.astype(jnp.float32).reshape(b, nc, GLA_CHUNK, h, t.shape[-1]), 1, 0)

    causal = jnp.tril(jnp.ones((GLA_CHUNK, GLA_CHUNK), dtype=bool))[None, :, :, None, None]

    def step(s, inp):
        qc, kc, vc, gc = inp
        cum = jnp.cumsum(gc, axis=1)
        o_inter = jnp.einsum('bchk,bhkv->bchv', qc * jnp.exp(cum), s)
        diff = cum[:, :, None] - cum[:, None, :]
        decay = jnp.exp(jnp.where(causal, diff, -jnp.inf))
        att = jnp.einsum('bihk,bjhk,bijhk->bhij', qc, kc, decay)
        o_intra = jnp.einsum('bhij,bjhv->bihv', att, vc)
        last = cum[:, -1]
        k_dec = kc * jnp.exp(last[:, None] - cum)
        s_new = jnp.exp(last)[..., None] * s + jnp.einsum('bchk,bchv->bhkv', k_dec, vc)
        return s_new, o_inter + o_intra

    q_scaled = q * GLA_DK ** -0.5
    s_final, o = lax.scan(step, state0, (to_chunks(q_scaled), to_chunks(k), to_chunks(v), to_chunks(log_a)))
    return jnp.moveaxis(o, 0, 1).reshape(b, l, h, v.shape[-1]), s_final


def gla_bidirectional(q, k, v, z, qc, kc, vc, zc, w_a2, b_a2):
    b = q.shape[0]
    o_lat = 0.0
    o_ctx = 0.0
    for d in range(2):
        la = gla_log_gate(z[:, :, d], w_a2[d], b_a2[d])
        lac = gla_log_gate(zc[:, :, d], w_a2[d], b_a2[d])
        lat = (q, k, v, la)
        cx = (qc, kc, vc, lac)
        if d == 1:
            lat = tuple(jnp.flip(t, axis=1) for t in lat)
            cx = tuple(jnp.flip(t, axis=1) for t in cx)
        s0 = jnp.zeros((b, GLA_HEADS, GLA_DK, GLA_DV), jnp.float32)
        oc, s_ctx = gla_scan(*cx, s0)
        ol, _ = gla_scan(*lat, s_ctx)
        if d == 1:
            ol = jnp.flip(ol, axis=1)
            oc = jnp.flip(oc, axis=1)
        o_lat = o_lat + ol
        o_ctx = o_ctx + oc
    return o_lat, o_ctx


def gla_output(o, r, g_gla):
    b, l = o.shape[:2]
    return (rms_norm(o, g_gla).reshape(b, l, GLA_WIDTH) * jax.nn.silu(r.astype(jnp.float32))).astype(r.dtype)


def moe_ffn(h, w_router, b_router, w_gu, b_gu, w_down, b_down):
    t, d = h.shape
    logits = (h @ w_router + b_router).astype(jnp.float32)
    top_logit, top_idx = lax.top_k(logits, TOP_K)
    gates = jax.nn.softmax(top_logit, axis=-1).astype(h.dtype)
    n_assign = t * TOP_K
    expert = top_idx.reshape(n_assign)
    token = jnp.repeat(jnp.arange(t, dtype=jnp.int32), TOP_K)
    gate = gates.reshape(n_assign)
    order = jnp.argsort(expert)
    expert_s, token_s, gate_s = expert[order], token[order], gate[order]
    counts = jnp.bincount(expert, length=N_EXPERTS)
    start = jnp.cumsum(counts) - counts
    padded = (counts + MOE_BLOCK - 1) // MOE_BLOCK * MOE_BLOCK
    pad_end = jnp.cumsum(padded)
    pad_start = pad_end - padded
    dest = pad_start[expert_s] + jnp.arange(n_assign) - start[expert_s]
    n_blocks = -(-(n_assign + N_EXPERTS * (MOE_BLOCK - 1)) // MOE_BLOCK)
    cap = n_blocks * MOE_BLOCK
    buf_token = jnp.zeros((cap,), jnp.int32).at[dest].set(token_s)
    buf_gate = jnp.zeros((cap,), h.dtype).at[dest].set(gate_s)
    block_expert = jnp.minimum(jnp.searchsorted(pad_end, jnp.arange(n_blocks) * MOE_BLOCK, side='right'),
                               N_EXPERTS - 1)

    def expert_block(acc, blk):
        tok, g, e = blk
        xb = h[tok]
        gu = xb @ w_gu[e] + b_gu[e]
        glu = jnp.minimum(gu[:, :D_EXPERT], SWIGLU_LIMIT)
        lin = jnp.clip(gu[:, D_EXPERT:], -SWIGLU_LIMIT, SWIGLU_LIMIT)
        act = glu * jax.nn.sigmoid(SWIGLU_ALPHA * glu) * (lin + 1)
        y = act @ w_down[e] + b_down[e]
        return acc.at[tok].add(y * g[:, None]), None

    out, _ = lax.scan(expert_block, jnp.zeros_like(h),
                      (buf_token.reshape(n_blocks, MOE_BLOCK), buf_gate.reshape(n_blocks, MOE_BLOCK), block_expert))
    return out


def hybrid_layer(x, ctx, c, c_ctx, w_ada, b_ada, g_mix, g_ffn, w_in, attn_sink, w_gate2, b_gate2,
                 g_gla, w_out, w_router, b_router, w_gu, b_gu, w_down, b_down, update_ctx):
    b, n, d = x.shape
    mod = jax.nn.silu(c) @ w_ada + b_ada
    mod_c = jax.nn.silu(c_ctx) @ w_ada + b_ada
    sh_a, sc_a, gt_a, sh_f, sc_f, gt_f = jnp.split(mod[:, None, :], 6, axis=-1)
    csh_a, csc_a, cgt_a, csh_f, csc_f, cgt_f = jnp.split(mod_c, 6, axis=-1)

    h = modulate(rms_norm(x, g_mix), sh_a, sc_a)
    hc = modulate(rms_norm(ctx, g_mix), csh_a, csc_a)
    q_a, k_a, v_a, q_g, k_g, v_g, r_g, z_g = split_projection(h @ w_in)
    qc_a, kc_a, vc_a, qc_g, kc_g, vc_g, rc_g, zc_g = split_projection(hc @ w_in)
    cos, sin = axial_rope_angles(n)
    q_a = apply_axial_rope(q_a, cos, sin)
    k_a = apply_axial_rope(k_a, cos, sin)
    attn_lat = window_attention(q_a, k_a, v_a, kc_a, vc_a, attn_sink)
    o_lat, o_ctx = gla_bidirectional(q_g, k_g, v_g, z_g, qc_g, kc_g, vc_g, zc_g, w_gate2, b_gate2)
    gla_lat = gla_output(o_lat, r_g, g_gla)
    x = x + gt_a * (jnp.concatenate([attn_lat, gla_lat], axis=-1) @ w_out)
    if update_ctx:
        attn_ctx = context_attention(qc_a, kc_a, vc_a, attn_sink)
        gla_ctx = gla_output(o_ctx, rc_g, g_gla)
        ctx = ctx + cgt_a * (jnp.concatenate([attn_ctx, gla_ctx], axis=-1) @ w_out)

    h = modulate(rms_norm(x, g_ffn), sh_f, sc_f).reshape(b * n, d)
    if update_ctx:
        hc = modulate(rms_norm(ctx, g_ffn), csh_f, csc_f).reshape(-1, d)
        y = moe_ffn(jnp.concatenate([h, hc], axis=0), w_router, b_router, w_gu, b_gu, w_down, b_down)
        ctx = ctx + cgt_f * y[b * n:].reshape(ctx.shape)
        y = y[:b * n]
    else:
        y = moe_ffn(h, w_router, b_router, w_gu, b_gu, w_down, b_down)
    x = x + gt_f * y.reshape(b, n, d)
    return x, ctx


def setup_inputs(seed: int = 0) -> dict:
    key = jax.random.key(seed)
    ks = jax.random.split(key, 21)
    d = D_MODEL
    L = DEPTH
    gk = GLA_HEADS * GLA_DK

    def nrm(k, shape, scale):
        return jax.random.normal(k, shape, jnp.float32) * scale

    return {
        'x': nrm(ks[0], (BATCH, SEQ, d), 1.0),
        'c': nrm(ks[1], (BATCH, d), 1.0),
        'ctx': nrm(ks[2], (BATCH, CTX_LEN, d), 1.0),
        'c_ctx': nrm(ks[3], (d,), 1.0),
        'w_ada': nrm(ks[4], (L, d, 6 * d), 0.5 * d ** -0.5),
        'b_ada': nrm(ks[5], (L, 6 * d), 0.01),
        'g_mix': 1.0 + nrm(ks[6], (L, d), 0.1),
        'g_ffn': 1.0 + nrm(ks[7], (L, d), 0.1),
        'w_in': nrm(ks[8], (L, d, IN_WIDTH), d ** -0.5),
        'attn_sink': nrm(ks[9], (L, ATTN_HEADS), 0.5),
        'w_gate2': nrm(ks[10], (L, 2, GLA_GATE_RANK, gk), GLA_GATE_RANK ** -0.5),
        'b_gate2': nrm(ks[11], (L, 2, gk), 0.1),
        'g_gla': 1.0 + nrm(ks[12], (L, GLA_DV), 0.1),
        'w_out': nrm(ks[13], (L, MIX_WIDTH, d), MIX_WIDTH ** -0.5),
        'w_router': nrm(ks[14], (L, d, N_EXPERTS), d ** -0.5),
        'b_router': nrm(ks[15], (L, N_EXPERTS), 0.01),
        'w_gu': nrm(ks[16], (L, N_EXPERTS, d, 2 * D_EXPERT), d ** -0.5),
        'b_gu': nrm(ks[17], (L, N_EXPERTS, 2 * D_EXPERT), 0.01),
        'w_down': nrm(ks[18], (L, N_EXPERTS, D_EXPERT, d), D_EXPERT ** -0.5),
        'b_down': nrm(ks[19], (L, N_EXPERTS, d), 0.01),
        'g_final': 1.0 + nrm(ks[20], (d,), 0.1),
    }


def reference(x, c, ctx, c_ctx, w_ada, b_ada, g_mix, g_ffn, w_in, attn_sink, w_gate2, b_gate2, g_gla,
              w_out, w_router, b_router, w_gu, b_gu, w_down, b_down, g_final):
    for layer in range(DEPTH):
        x, ctx = hybrid_layer(x, ctx, c, c_ctx, w_ada[layer], b_ada[layer], g_mix[layer], g_ffn[layer],
                              w_in[layer], attn_sink[layer], w_gate2[layer], b_gate2[layer], g_gla[layer],
                              w_out[layer], w_router[layer], b_router[layer], w_gu[layer], b_gu[layer],
                              w_down[layer], b_down[layer], update_ctx=layer < DEPTH - 1)
    return rms_norm(x, g_final)
```

```python
import numpy as np
import ml_dtypes
from contextlib import ExitStack
import concourse.bass as bass
import concourse.mybir as mybir
from concourse.bass_utils import run_bass_kernel_spmd

F32 = mybir.dt.float32; BF16 = mybir.dt.bfloat16
AF = mybir.ActivationFunctionType; ALU = mybir.AluOpType; AX = mybir.AxisListType
D = 2048; KC = 16; CTX = 256; GRID_W = 64; EPS = 1e-6
NEG = -30000.0


class Sched:
    def __init__(self, nc):
        self.nc = nc; self.ops = []; self.last_w = {}; self.readers = {}; self.bar = 0; self.fam = set()
    def add(self, eng, fn, reads=(), writes=(), dma=False, semkey=None, nd=1):
        idx = len(self.ops); deps = set()
        for b in reads:
            if b in self.last_w: deps.add(self.last_w[b])
        for b in writes:
            if b in self.last_w: deps.add(self.last_w[b])
            deps.update(self.readers.get(b, ()))
        deps.discard(idx)
        if dma and semkey is None: semkey = ("dma", writes[0])
        self.ops.append(dict(eng=eng, fn=fn, deps=deps, dma=dma, semkey=semkey, users=0, nd=nd, bar=self.bar, force=False))
        for b in reads: self.readers.setdefault(b, []).append(idx)
        for b in writes: self.last_w[b] = idx; self.readers[b] = []
        return idx
    def barrier(self):
        seen = set()
        for o in reversed(self.ops):
            if o["dma"] or o["fn"] is None or o["eng"] in seen: continue
            seen.add(o["eng"]); o["force"] = True
        self.bar += 1
    def plan(self, final_keys):
        self.add("sync", None, reads=list(final_keys))
        ops = self.ops
        def pe_pe(a, b): return a["eng"] == "tensor" and b["eng"] == "tensor" and not a["dma"] and not b["dma"]
        for o in ops:
            for d in o["deps"]:
                if not pe_pe(ops[d], o): ops[d]["users"] += 1
        semkeys = {}; cnt = {}; snaps = {}; waited = {}; engbar = {}
        def semid(k):
            if k not in semkeys: semkeys[k] = len(semkeys)
            return semkeys[k]
        for o in ops:
            if o["bar"] not in snaps: snaps[o["bar"]] = dict(cnt)
            w = {}
            if engbar.get(o["eng"], 0) < o["bar"]:
                engbar[o["eng"]] = o["bar"]
                for k, v in snaps[o["bar"]].items(): w[k] = v
            for d in o["deps"]:
                p = ops[d]
                if pe_pe(p, o) or p["sig"] is None: continue
                v = cnt[p["sig"]] if (p["dma"] and p["semkey"] in self.fam) else p["sigval"]
                w[p["sig"]] = max(w.get(p["sig"], 0), v)
            o["waits"] = []
            for k, v in sorted(w.items()):
                if v > 0 and waited.get((o["eng"], k), 0) < v:
                    waited[(o["eng"], k)] = v; o["waits"].append((k, v))
            o["sig"] = None
            if o["fn"] is None: continue
            if o["dma"]:
                k = semid(o["semkey"]); cnt[k] = cnt.get(k, 0) + 16 * o["nd"]; o["sig"] = k; o["sigval"] = cnt[k]
            elif o["users"] > 0 or o["force"]:
                k = semid(("eng", o["eng"])); cnt[k] = cnt.get(k, 0) + 1; o["sig"] = k; o["sigval"] = cnt[k]
        return len(semkeys)
    def emit(self, block, sems):
        ops = self.ops
        def run(engname, eng):
            for o in ops:
                if o["eng"] != engname: continue
                for k, v in o["waits"]: eng.wait_ge(sems[k], v)
                if o["fn"] is None: continue
                r = o["fn"](eng)
                if o["dma"]:
                    for ins in r: ins.then_inc(sems[o["sig"]], 16)
                elif o["sig"] is not None:
                    r.then_inc(sems[o["sig"]], 1)
        @block.sync
        def _(e): run("sync", e)
        @block.scalar
        def _(e): run("scalar", e)
        @block.vector
        def _(e): run("vector", e)
        @block.gpsimd
        def _(e): run("gpsimd", e)
        @block.tensor
        def _(e): run("tensor", e)


class Pool:
    def __init__(self, es, nc, name, n, shape, dt, psum=False):
        self.t = [es.enter_context((nc.psum_tensor if psum else nc.sbuf_tensor)(f"{name}{i}", shape, dt)) for i in range(n)]
        self.k = [f"{name}{i}" for i in range(n)]; self.i = 0
    def next(self):
        j = self.i % len(self.t); self.i += 1
        return self.t[j], self.k[j]


class _Stop(Exception):
    pass


def build(SEQ, NE, debug=False, stop=99, cut=99, ocut=99, burn=0):
    TL = SEQ // 2; NT = TL // 128; NTA = SEQ // 128; U = SEQ + CTX; CT0 = SEQ // 128
    nc = bass.Bass("TRN2", target_bir_lowering=False)
    def din(name, shape, dt=F32): return nc.dram_tensor(name, list(shape), dt, kind="ExternalInput").ap()
    def dscr(name, shape, dt): return nc.dram_tensor(name, list(shape), dt, kind=("ExternalOutput" if debug else "Internal")).ap()
    xall = din("xall", [SEQ, D]); ctxl = din("ctxl", [CTX, D]); cvec = din("cvec", [2, D])
    w_ada = din("w_ada", [D, 6 * D]); b_ada = din("b_ada", [6 * D]); g_mix = din("g_mix", [D]); g_ffn = din("g_ffn", [D])
    g_final = din("g_final", [D]); w_in = din("w_in", [D, 4640]); w_qks = din("w_qks", [D, 1280]); w_z = din("w_z", [D, 32])
    attn_sink = din("attn_sink", [16]); w_g2 = din("w_g2", [2, 16, 512]); b_g2 = din("b_g2", [2, 512]); g_gla = din("g_gla", [256])
    w_out = din("w_out", [D, D]); w_router = din("w_router", [D, NE]); b_router = din("b_router", [NE])
    c_identf = din("c_identf", [128, 128]); c_identb = din("c_identb", [128, 128], BF16)
    c_cos = din("c_cos", [128, TL + 128]); c_sin = din("c_sin", [128, TL + 128])
    c_maskp = din("c_maskp", [128, 512], BF16); c_maskn = din("c_maskn", [128, 512], BF16)
    c_tri = din("c_tri", [2, 128, 128]); c_rev = din("c_rev", [2, 128, 128]); c_gm = din("c_gm", [2, 128, 128])
    out = nc.dram_tensor("out", [TL, D], F32, kind="ExternalOutput").ap()
    hT = dscr("s_hT", [128, KC, U], BF16); qT = dscr("s_qT", [64, 16, TL], BF16); kT = dscr("s_kT", [64, 4, U], BF16)
    qgT = dscr("s_qgT", [128, 4, TL], BF16); kgT = dscr("s_kgT", [128, 4, TL], BF16); rT = dscr("s_rT", [128, 8, TL], BF16)
    vA = dscr("s_vA", [U, 256], BF16); kG = dscr("s_kG", [U, 512], BF16); vG = dscr("s_vG", [U, 1024], BF16)
    sp = [dscr("s_spA", [U, 512], F32), dscr("s_spB", [U, 512], F32)]
    oAT = dscr("s_oAT", [128, 8, TL], F32); cTa = dscr("s_cTa", [64, 16, TL], BF16); cTg = dscr("s_cTg", [128, 8, TL], BF16)
    x1s = dscr("s_x1", [TL, D], F32); h2T = dscr("s_h2T", [128, KC, TL], BF16)

    es = ExitStack()
    S = Sched(nc)
    for f in ["hT", "qT", "kT", "qgT", "kgT", "rT", "vA", "kG", "vG", "spA", "spB", "oAT", "cTa", "cTg", "x1s", "h2T", "out"]:
        S.fam.add(("st", f))
    sb = lambda name, shape, dt, ctx=es: ctx.enter_context(nc.sbuf_tensor(name, shape, dt))
    def dma(eng, o, i, reads, writes, semkey=None, slow=False):
        if slow: S.add(eng, lambda e: [e.dma_start(out=o, in_=i, allow_slow_non_contiguous=True)], reads, writes, dma=True, semkey=semkey)
        else: S.add(eng, lambda e: [e.dma_start(out=o, in_=i)], reads, writes, dma=True, semkey=semkey)
    def load(o, i, key, reads=(), eng="sync"): dma(eng, o, i, list(reads), [key])
    def store(o, i, fam, key, reads): dma("gpsimd", o, i, list(reads), [key], semkey=("st", fam))
    def act(o, i, func, reads, writes, **kw): S.add("scalar", lambda e: e.activation(out=o, in_=i, func=func, **kw), reads, writes)
    def vtt(o, a, b, op, reads, writes, eng="vector"): S.add(eng, lambda e: e.tensor_tensor(out=o, in0=a, in1=b, op=op), reads, writes)
    def vts(o, a, s1, s2, op0, op1, reads, writes, eng="vector"):
        if op1 is None: S.add(eng, lambda e: e.tensor_scalar(out=o, in0=a, scalar1=s1, scalar2=None, op0=op0), reads, writes)
        else: S.add(eng, lambda e: e.tensor_scalar(out=o, in0=a, scalar1=s1, scalar2=s2, op0=op0, op1=op1), reads, writes)
    def vstt(o, a, s, b, op0, op1, reads, writes): S.add("vector", lambda e: e.scalar_tensor_tensor(out=o, in0=a, scalar=s, in1=b, op0=op0, op1=op1), reads, writes)
    def vcopy(o, i, reads, writes, eng="vector"): S.add(eng, lambda e: e.tensor_copy(out=o, in_=i), reads, writes)
    def vrecip(o, i, reads, writes): S.add("vector", lambda e: e.reciprocal(out=o, in_=i), reads, writes)
    def mm(o, pairs, reads, writes, start=True, stop=True):
        def f(e):
            n = len(pairs)
            for j, (l, r) in enumerate(pairs):
                ins = e.matmul(o, lhsT=l, rhs=r, start=(start and j == 0), stop=(stop and j == n - 1))
            return ins
        S.add("tensor", f, reads, writes)
    def tr(o, i, ident, reads, writes): S.add("tensor", lambda e: e.transpose(out=o, in_=i, identity=ident), reads, writes)
    evq = [0]
    def evac(o, i, reads, writes):
        evq[0] += 1
        if evq[0] % 2: act(o, i, AF.Copy, reads, writes)
        else: vcopy(o, i, reads, writes)

    PS = Pool(es, nc, "ps", 8, [128, 512], F32, psum=True)
    identf = sb("identf", [128, 128], F32); identb = sb("identb", [128, 128], BF16)
    onesf = sb("onesf", [128, 128], F32); onesb = sb("onesb", [128, 64], BF16)
    GTF = sb("GTF", [128, D], F32); GFIN = sb("GFIN", [128, D], F32)
    GATES = sb("GATES", [128, NT, NE], F32)
    sm = sb("sm", [128, 64], F32)
    eA = ExitStack()
    GTA = sb("GTA", [128, D], F32, eA); A2 = sb("A2", [128, D], F32, eA); B2 = sb("B2", [128, D], F32, eA)
    load(identf[:], c_identf[:, :], "identf"); load(identb[:], c_identb[:, :], "identb")
    S.add("vector", lambda e: e.memset(onesf[:], 1.0), [], ["onesf"]); S.add("vector", lambda e: e.memset(onesb[:], 1.0), [], ["onesb"])
    load(GFIN[:], g_final.partition_broadcast(128), "GFIN")

    def rstd_from_ss(ssk, n, tag):
        a = sm[:, ssk + 1:ssk + 2]; b = sm[:, ssk + 2:ssk + 3]; r = sm[:, ssk + 3:ssk + 4]
        vts(a, sm[:, ssk:ssk + 1], 1.0 / n, EPS, ALU.mult, ALU.add, [f"sm{ssk}"], [f"sm{ssk+1}"])
        act(b, a, AF.Sqrt, [f"sm{ssk+1}"], [f"sm{ssk+2}"])
        vrecip(r, b, [f"sm{ssk+2}"], [f"sm{ssk+3}"])
        return r, f"sm{ssk+3}"

    phase = [0]
    def chk():
        phase[0] += 1
        return phase[0] >= stop
    def body():
        with ExitStack() as e1:
            A1 = sb("A1", [128, D], F32, e1); B1 = sb("B1", [128, D], F32, e1); A1c = sb("A1c", [128, D], F32, e1); B1c = sb("B1c", [128, D], F32, e1)
            with ExitStack() as e0:
                cv = sb("cv", [2, D], F32, e0); ccol = sb("ccol", [128, 32], F32, e0)
                Lc = sb("Lc", [128, KC, 128], F32, e0); Lx = sb("Lx", [128, KC, 128], F32, e0)
                WA = Pool(e0, nc, "wa", 2, [128, KC, 512], F32); BB = Pool(e0, nc, "bb", 2, [128, 512], F32)
                GM = sb("GM", [128, D], F32, e0)
                load(cv[:], cvec[:, :], "cv")
                act(cv[:], cv[:], AF.Silu, ["cv"], ["cv"])
                p, pk = PS.next()
                for kc in range(KC):
                    tr(p[:, 2 * kc:2 * kc + 2], cv[0:2, kc * 128:(kc + 1) * 128], identf[0:2, 0:2], ["cv", "identf"], [pk])
                vcopy(ccol[:], p[:, 0:32], [pk], ["ccol"])
                for kc in range(KC):
                    vcopy(Lc[:, kc, :], ccol[:, 2 * kc:2 * kc + 1].to_broadcast([128, 128]), ["ccol"], ["Lc"])
                    vcopy(Lx[:, kc, :], ccol[:, 2 * kc + 1:2 * kc + 2].to_broadcast([128, 128]), ["ccol"], ["Lx"])
                dst = [(B1, "B1"), (A1, "A1"), (GTA, "GTA"), (B2, "B2"), (A2, "A2"), (GTF, "GTF")]
                dstc = [(B1c, "B1c"), (A1c, "A1c")]
                for j in range(6):
                    for n in range(4):
                        wa, wk = WA.next(); bb, bk = BB.next(); c0 = j * D + n * 512
                        load(wa[:], w_ada[:, c0:c0 + 512].rearrange("(kc p) c -> p kc c", p=128), wk)
                        load(bb[:], b_ada[c0:c0 + 512].partition_broadcast(128), bk)
                        for (L, Lk, dd) in ([(Lc, "Lc", dst[j])] + ([(Lx, "Lx", dstc[j])] if j < 2 else [])):
                            p, pk = PS.next()
                            mm(p[:], [(L[:, kc, :], wa[:, kc, :]) for kc in range(KC)], [Lk, wk], [pk])
                            vtt(dd[0][:, n * 512:(n + 1) * 512], p[:], bb[:], ALU.add, [pk, bk], [dd[1]])
                for (g, A, Ak) in [(g_mix, A1, "A1"), (g_mix, A1c, "A1c"), (g_ffn, A2, "A2")]:
                    load(GM[:], g.partition_broadcast(128), "GM")
                    vstt(A[:], A[:], 1.0, GM[:], ALU.add, ALU.mult, [Ak, "GM"], [Ak])
            S.barrier()
            if chk(): return
            with ExitStack() as e0:
                XT = Pool(e0, nc, "xt", 2, [128, D], F32); TMP = sb("ntmp", [128, D], F32, e0); JK = sb("njunk", [128, D], BF16, e0)
                HB = Pool(e0, nc, "hb", 2, [128, D], BF16); HTT = Pool(e0, nc, "htt", 2, [128, KC, 128], BF16)
                for tau in range(NTA + 2):
                    isctx = tau >= NTA
                    src = ctxl[(tau - NTA) * 128:(tau - NTA + 1) * 128, :] if isctx else xall[tau * 128:(tau + 1) * 128, :]
                    A, Ak, B, Bk = (A1c, "A1c", B1c, "B1c") if isctx else (A1, "A1", B1, "B1")
                    xt, xk = XT.next(); hb, hk = HB.next(); htt, htk = HTT.next()
                    load(xt[:], src, xk)
                    act(JK[:], xt[:], AF.Square, [xk], ["njunk", "sm0"], accum_out=sm[:, 0:1])
                    r, rk = rstd_from_ss(0, D, "n")
                    vstt(TMP[:], xt[:], r, A[:], ALU.mult, ALU.mult, [xk, rk, Ak], ["ntmp"])
                    vtt(hb[:], TMP[:], B[:], ALU.add, ["ntmp", Bk], [hk])
                    for half in range(2):
                        p, pk = PS.next(); pb = p[:].bitcast(BF16)
                        for j in range(8):
                            kc = half * 8 + j
                            tr(pb[:, j * 128:(j + 1) * 128], hb[:, kc * 128:(kc + 1) * 128], identb[:], [hk, "identb"], [pk])
                        evac(htt[:, half * 8:(half + 1) * 8, :], pb[:, 0:1024].rearrange("p (a b) -> p a b", a=8), [pk], [htk])
                    store(hT[:, :, tau * 128:(tau + 1) * 128], htt[:], "hT", ("hT", tau), [htk])
            S.barrier()
            if chk(): return

        def tokblocks(ranges):
            bl = []
            for (a, b) in ranges:
                t = a
                while t < b:
                    n = min(512, b - t); bl.append((t, n)); t += n
            return bl
        def hkeys(t0, n): return [("hT", t) for t in range(t0 // 128, (t0 + n + 127) // 128)]
        with ExitStack() as e1:
            WB = sb("gWB", [128, KC, 1024], BF16, e1)
            STG = Pool(e1, nc, "gstg", 2, [128, KC, 128], F32)
            HBK = Pool(e1, nc, "ghb", 2, [128, KC, 512], BF16)
            def load_w(col0, src, ncols, dup=False):
                c = 0
                while c < ncols:
                    n = min(128, ncols - c)
                    st, sk = STG.next()
                    load(st[:, :, 0:n], src[:, c:c + n].rearrange("(kc p) c -> p kc c", p=128), sk)
                    if not dup:
                        act(WB[:, :, col0 + c:col0 + c + n], st[:, :, 0:n], AF.Copy, [sk], ["gWB"])
                    else:
                        for q in range(n // 64):
                            for r2 in range(2):
                                o0 = col0 + 2 * (c + q * 64) + r2 * 64
                                act(WB[:, :, o0:o0 + 64], st[:, :, q * 64:(q + 1) * 64], AF.Copy, [sk], ["gWB"])
                    c += n
            def load_h(t0, n):
                hb, hk = HBK.next()
                load(hb[:, :, 0:n], hT[:, :, t0:t0 + n], hk, reads=hkeys(t0, n))
                return hb, hk
            def fm_chunk(hb, hk, n, wcol, M=128):
                p, pk = PS.next()
                mm(p[0:M, 0:n], [(WB[:, kc, wcol:wcol + M], hb[:, kc, 0:n]) for kc in range(KC)], ["gWB", hk], [pk])
                return p, pk
            with ExitStack() as e2:
                COS = Pool(e2, nc, "gcos", 2, [128, 512], F32); SIN = Pool(e2, nc, "gsin", 2, [128, 512], F32)
                T1 = Pool(e2, nc, "gt1", 2, [128, 512], F32); T2 = Pool(e2, nc, "gt2", 2, [128, 512], F32)
                OB = Pool(e2, nc, "gob", 2, [128, 8, 512], BF16)
                qTv = qT.rearrange("p (c two) t -> p two c t", two=2); kTv = kT.rearrange("p (c two) t -> p two c t", two=2)
                for which in range(3):
                    if which < 2:
                        rr = which
                        load_w(0, w_in[:, rr * 512:(rr + 1) * 512], 512); load_w(512, w_qks[:, rr * 512:(rr + 1) * 512], 512)
                        dstv = qTv; fam = "qT"; ch0 = rr * 4; nch = 4
                        blocks = [(t, n, True) for (t, n) in tokblocks([(0, TL)])]
                    else:
                        load_w(0, w_in[:, 1024:1280], 256); load_w(512, w_qks[:, 1024:1280], 256)
                        dstv = kTv; fam = "kT"; ch0 = 0; nch = 2
                        blocks = [(t, n, True) for (t, n) in tokblocks([(0, TL + 128)])] + [(SEQ, 256, False)]
                    for (t0, n, rope) in blocks:
                        hb, hk = load_h(t0, n)
                        if rope:
                            cs, ck = COS.next(); sn, snk = SIN.next()
                            load(cs[:, 0:n], c_cos[:, t0:t0 + n], ck); load(sn[:, 0:n], c_sin[:, t0:t0 + n], snk)
                        ob, obk = OB.next()
                        for c in range(nch):
                            p, pk = fm_chunk(hb, hk, n, c * 128)
                            if rope:
                                p2, pk2 = fm_chunk(hb, hk, n, 512 + c * 128)
                                t1, t1k = T1.next(); t2, t2k = T2.next()
                                vtt(t1[:, 0:n], p[:, 0:n], cs[:, 0:n], ALU.mult, [pk, ck], [t1k])
                                vtt(t2[:, 0:n], p2[:, 0:n], sn[:, 0:n], ALU.mult, [pk2, snk], [t2k])
                                vtt(ob[:, c, 0:n], t1[:, 0:n], t2[:, 0:n], ALU.add, [t1k, t2k], [obk])
                            else:
                                evac(ob[:, c, 0:n], p[:, 0:n], [pk], [obk])
                        for two in range(2):
                            store(dstv[:, two, ch0:ch0 + nch, t0:t0 + n], ob[two * 64:(two + 1) * 64, 0:nch, 0:n], fam, (fam, which, t0, two), [obk])
                load_w(0, w_in[:, 1536:2560], 1024)
                for (t0, n) in tokblocks([(0, TL)]):
                    hb, hk = load_h(t0, n)
                    for (dstT, fam, c0) in [(qgT, "qgT", 0), (kgT, "kgT", 4)]:
                        ob, obk = OB.next()
                        for c in range(4):
                            p, pk = fm_chunk(hb, hk, n, (c0 + c) * 128)
                            evac(ob[:, c, 0:n], p[:, 0:n], [pk], [obk])
                        store(dstT[:, :, t0:t0 + n], ob[:, 0:4, 0:n], fam, (fam, t0), [obk])
                load_w(0, w_in[:, 3584:4608], 1024)
                for (t0, n) in tokblocks([(0, TL)]):
                    hb, hk = load_h(t0, n)
                    ob, obk = OB.next()
                    for c in range(8):
                        p, pk = fm_chunk(hb, hk, n, c * 128)
                        evac(ob[:, c, 0:n], p[:, 0:n], [pk], [obk])
                    store(rT[:, :, t0:t0 + n], ob[:, :, 0:n], "rT", ("rT", t0), [obk])
            S.barrier()
            if chk(): return
            with ExitStack() as e2:
                load_w(0, w_z[:, 0:32], 32)
                wg2 = sb("gwg2", [16, 2, 512], F32, e2); bg2 = sb("gbg2", [128, 2, 512], F32, e2)
                ZD = Pool(e2, nc, "gzd", 2, [16, 2, 512], F32); UU = Pool(e2, nc, "guu", 2, [128, 512], F32); SPT = Pool(e2, nc, "gspt", 2, [128, 512], F32)
                onecol = sb("gone", [128, 1], F32, e2)
                S.add("vector", lambda e: e.memset(onecol[:], 1.0), [], ["gone"])
                load(wg2[:], w_g2.rearrange("d k n -> k d n"), "gwg2")
                for d in range(2): load(bg2[:, d, :], b_g2[d].partition_broadcast(128), "gbg2")
                for (t0, n) in tokblocks([(0, SEQ), (SEQ, SEQ + CTX)]):
                    hb, hk = load_h(t0, n)
                    zd, zk = ZD.next()
                    for d in range(2):
                        p, pk = fm_chunk(hb, hk, n, d * 16, M=16)
                        evac(zd[:, d, 0:n], p[0:16, 0:n], [pk], [zk])
                    for i in range(n // 128):
                        for d in range(2):
                            if d == 0 and NT <= (t0 // 128 + i) < NTA: continue
                            p, pk = PS.next()
                            mm(p[:], [(zd[0:16, d, i * 128:(i + 1) * 128], wg2[0:16, d, :])], [zk, "gwg2"], [pk])
                            uu, uk = UU.next(); spt, spk = SPT.next()
                            vtt(uu[:], p[:], bg2[:, d, :], ALU.add, [pk, "gbg2"], [uk])
                            act(uu[:], uu[:], AF.Exp, [uk], [uk], scale=-1.0)
                            act(spt[:], uu[:], AF.Ln, [uk, "gone"], [spk], bias=onecol[:, 0:1])
                            r0 = t0 + i * 128
                            store(sp[d][r0:r0 + 128, :], spt[:], "spA" if d == 0 else "spB", ("sp", d, r0 // 128), [spk])
                TMO = Pool(e2, nc, "gtmo", 2, [128, 1024], BF16)
                for rnd in range(2):
                    if rnd == 0: load_w(0, w_in[:, 1280:1536], 256); load_w(256, w_in[:, 2048:2560], 512)
                    else: load_w(0, w_in[:, 2560:3584], 1024)
                    for (t0, n) in tokblocks([(0, SEQ), (SEQ, SEQ + CTX)]):
                        hb, hk = load_h(t0, n)
                        for i in range(n // 128):
                            tau = t0 // 128 + i; r0 = tau * 128
                            need_va = (tau <= NT) or (tau >= NTA)
                            tmo, tk = TMO.next()
                            groups = (([(0, 256)] if need_va else []) + [(256, 512)]) if rnd == 0 else [(0, 512), (512, 512)]
                            for (c0, cn) in groups:
                                p, pk = PS.next()
                                mm(p[:, 0:cn], [(hb[:, kc, i * 128:(i + 1) * 128], WB[:, kc, c0:c0 + cn]) for kc in range(KC)], ["gWB", hk], [pk])
                                evac(tmo[:, c0:c0 + cn], p[:, 0:cn], [pk], [tk])
                            if rnd == 0:
                                if need_va: store(vA[r0:r0 + 128, :], tmo[:, 0:256], "vA", ("vA", tau), [tk])
                                store(kG[r0:r0 + 128, :], tmo[:, 256:768], "kG", ("kG", tau), [tk])
                            else:
                                store(vG[r0:r0 + 128, :], tmo[:, 0:1024], "vG", ("vG", tau), [tk])
        S.barrier()
        if chk(): return

        with ExitStack() as e1:
            maskp = sb("amp", [128, 512], BF16, e1); maskn = sb("amn", [128, 512], BF16, e1); esink = sb("aes", [128, 16], F32, e1)
            kctx = sb("akc", [64, 4, 256], BF16, e1); vctx = sb("avc", [128, 2, 256], BF16, e1)
            QT = Pool(e1, nc, "aqt", 2, [64, 16, 128], BF16); KW = Pool(e1, nc, "akw", 2, [64, 4, 384], BF16); VW = Pool(e1, nc, "avw", 2, [128, 3, 256], BF16)
            PT = Pool(e1, nc, "apt", 2, [128, 5, 512], BF16); DEN = Pool(e1, nc, "aden", 2, [64, 512], F32); AO = Pool(e1, nc, "aao", 2, [64, 16, 128], BF16)
            load(maskp[:], c_maskp[:, :], "amp"); load(maskn[:], c_maskn[:, :], "amn")
            load(esink[:], attn_sink.partition_broadcast(128), "aes"); act(esink[:], esink[:], AF.Exp, ["aes"], ["aes"])
            load(kctx[:], kT[:, :, SEQ:SEQ + 256], "akc", reads=[("kT", 2, SEQ, two) for two in range(2)])
            for j in range(2): load(vctx[:, j, :], vA[SEQ + j * 128:SEQ + (j + 1) * 128, :], "avc", reads=[("vA", CT0 + j)])
            for i in range(NT):
                lo = max(i - 1, 0); nb = (i + 2) - lo
                qt, qk = QT.next(); kw, kk = KW.next(); vw, vk = VW.next(); ao, aok = AO.next()
                load(qt[:], qT[:, :, i * 128:(i + 1) * 128], qk, reads=[("qT", w_, (i * 128) // 512 * 512, two) for w_ in range(2) for two in range(2)])
                load(kw[:, :, 0:nb * 128], kT[:, :, lo * 128:(lo + nb) * 128], kk, reads=[("kT", 2, (t * 128) // 512 * 512, two) for t in range(lo, lo + nb) for two in range(2)])
                for t in range(nb): load(vw[:, t, :], vA[(lo + t) * 128:(lo + t + 1) * 128, :], vk, reads=[("vA", lo + t)])
                blks = []
                for t in range(nb):
                    kt_ = lo + t
                    msk = (maskp, "amp") if kt_ == i - 1 else ((maskn, "amn") if kt_ == i + 1 else None)
                    blks.append((lambda j, t=t, kw=kw: kw[:, j, t * 128:(t + 1) * 128], lambda j, t=t, vw=vw: vw[:, t, j * 64:(j + 1) * 64], msk))
                for c in range(2):
                    blks.append((lambda j, c=c: kctx[:, j, c * 128:(c + 1) * 128], lambda j, c=c: vctx[:, c, j * 64:(j + 1) * 64], None))
                for j in range(4 if cut >= 2 else 0):
                    pt, ptk = PT.next()
                    for bi, (kget, vget, msk) in enumerate(blks):
                        p, pk = PS.next()
                        def f(e, p=p, kap=kget(j), msk=msk, qap=qt[:, 4 * j:4 * j + 4, :]):
                            if msk is not None: e.matmul(p[:], lhsT=identb[:], rhs=msk[0][:], start=True, stop=False)
                            return e.matmul(p[:], lhsT=kap, rhs=qap, start=(msk is None), stop=True)
                        S.add("tensor", f, [qk, kk, "akc", "identb"] + ([msk[1]] if msk else []), [pk])
                        act(pt[:, bi, :], p[:], AF.Exp, [pk], [ptk], scale=0.125)
                    nbk = len(blks)
                    if cut < 3: continue
                    pa, pak = PS.next(); pd, pdk = PS.next()
                    mm(pa[0:64, :], [(vget(j), pt[:, bi, :]) for bi, (kget, vget, msk) in enumerate(blks)], [ptk, vk, "avc"], [pak])
                    mm(pd[0:64, :], [(onesb[:, 0:64], pt[:, bi, :]) for bi in range(nbk)], [ptk, "onesb"], [pdk])
                    if cut < 4: continue
                    den, dk = DEN.next()
                    heads = [4 * j, 4 * j + 1, 4 * j + 2, 4 * j + 3]
                    for q, h in enumerate(heads):
                        vts(den[:, q * 128:(q + 1) * 128], pd[0:64, q * 128:(q + 1) * 128], esink[0:64, h:h + 1], None, ALU.add, None, [pdk, "aes"], [dk])
                    vrecip(den[:], den[:], [dk], [dk])
                    for q, h in enumerate(heads):
                        vtt(ao[:, h, :], pa[0:64, q * 128:(q + 1) * 128], den[:, q * 128:(q + 1) * 128], ALU.mult, [pak, dk], [aok])
                if cut >= 5: store(cTa[:, :, i * 128:(i + 1) * 128], ao[:], "cTa", ("cTa", i), [aok])
        S.barrier()
        if chk(): return

        with ExitStack() as e1:
            Sf = [sb(f"lSf{h}", [128, 256], F32, e1) for h in range(4)]; Sb = [sb(f"lSb{h}", [128, 256], BF16, e1) for h in range(4)]
            tri = sb("ltri", [128, 2, 128], F32, e1); rev = sb("lrev", [128, 2, 128], F32, e1); gmk = sb("lgm", [128, 2, 128], F32, e1)
            ggc = sb("lgg", [128, 2], F32, e1); cm = sb("lcm", [128, 2], F32, e1)
            S.add("vector", lambda e: e.memset(cm[:], 0.0), [], ["lcm"])
            S.add("vector", lambda e: e.memset(cm[0:64, 0:1], 1.0), [], ["lcm"]); S.add("vector", lambda e: e.memset(cm[64:128, 1:2], 1.0), [], ["lcm"])
            for d in range(2):
                load(tri[:, d, :], c_tri[d], "ltri"); load(rev[:, d, :], c_rev[d], "lrev"); load(gmk[:, d, :], c_gm[d], "lgm")
            dma("sync", ggc[:], g_gla.rearrange("(v p) -> p v", p=128), [], ["lgg"], slow=True)
            SPL = Pool(e1, nc, "lsp", 2, [128, 512], F32); KGL = Pool(e1, nc, "lkg", 2, [128, 512], BF16); VGL = Pool(e1, nc, "lvg", 2, [128, 1024], BF16)
            QGL = Pool(e1, nc, "lqg", 2, [128, 4, 128], BF16); KTL = Pool(e1, nc, "lkt", 2, [128, 4, 128], BF16)
            RTL = Pool(e1, nc, "lrt", 2, [128, 8, 128], BF16); OAL = Pool(e1, nc, "loa", 2, [128, 8, 128], F32)
            ELM = Pool(e1, nc, "lelm", 2, [128, 512], F32); KDC = Pool(e1, nc, "lkd", 2, [128, 2, 512], BF16)
            EE = Pool(e1, nc, "lee", 3, [128, 128], F32); EI = Pool(e1, nc, "lei", 2, [128, 128], F32)
            QQ = Pool(e1, nc, "lqq", 2, [128, 128], BF16); KK = Pool(e1, nc, "lkk", 2, [128, 128], BF16); AM = Pool(e1, nc, "lam", 2, [128, 128], BF16)
            OT = Pool(e1, nc, "lot", 2, [128, 8, 128], F32); GO = Pool(e1, nc, "lgo", 2, [128, 8, 128], BF16)
            OS = Pool(e1, nc, "los", 2, [128, 2, 128], F32); SQ = Pool(e1, nc, "lsq", 2, [128, 2, 128], F32); SIL = Pool(e1, nc, "lsil", 2, [128, 2, 128], F32)
            RS = Pool(e1, nc, "lrs", 2, [128, 128], F32); TT = Pool(e1, nc, "ltt", 2, [128, 128], F32)
            for d in range(2):
                for h in range(4):
                    S.add("vector", lambda e, h=h: e.memset(Sf[h][:], 0.0), [], [f"lSf{h}"])
                    S.add("vector", lambda e, h=h: e.memset(Sb[h][:], 0.0), [], [f"lSb{h}"])
                if d == 0: seq = [(CT0, False), (CT0 + 1, False)] + [(t, True) for t in range(NT)]
                else: seq = [(CT0 + 1, False), (CT0, False)] + [(t, False) for t in range(NTA - 1, NT - 1, -1)] + [(t, True) for t in range(NT - 1, -1, -1)]
                co = [0, 1] if d == 0 else [1, 0]
                for (tau, full) in seq:
                    r0 = tau * 128
                    spt, spk = SPL.next(); kg, kgk = KGL.next(); vg, vgk = VGL.next()
                    load(spt[:], sp[d][r0:r0 + 128, :], spk, reads=[("sp", d, tau)])
                    load(kg[:], kG[r0:r0 + 128, :], kgk, reads=[("kG", tau)]); load(vg[:], vG[r0:r0 + 128, :], vgk, reads=[("vG", tau)])
                    if full:
                        qg, qgk = QGL.next(); kt, ktk = KTL.next(); b5 = (r0 // 512) * 512
                        load(qg[:], qgT[:, :, r0:r0 + 128], qgk, reads=[("qgT", b5)]); load(kt[:], kgT[:, :, r0:r0 + 128], ktk, reads=[("kgT", b5)])
                        if d == 1:
                            rt, rtk = RTL.next(); oa, oak = OAL.next(); go, gok = GO.next()
                            load(rt[:], rT[:, :, r0:r0 + 128], rtk, reads=[("rT", b5)]); load(oa[:], oAT[:, :, r0:r0 + 128], oak, reads=[("oAT", tau)])
                        else:
                            ot, otk = OT.next()
                    p, pk = PS.next()
                    mm(p[:], [(rev[:, d, :], spt[:])], ["lrev", spk], [pk])
                    elm, ek = ELM.next(); kd, kdk = KDC.next()
                    act(elm[:], p[:], AF.Exp, [pk], [ek])
                    for c2 in range(2): vstt(kd[:, c2, :], kg[:], cm[:, c2:c2 + 1], elm[:], ALU.mult, ALU.mult, [kgk, ek, "lcm"], [kdk])
                    for h in range(4):
                        pc, pck = PS.next()
                        mm(pc[:, 0:128], [(spt[:, h * 128:(h + 1) * 128], tri[:, d, :])], [spk, "ltri"], [pck])
                        ee, eek = EE.next()
                        act(ee[:], pc[:, 0:128], AF.Exp, [pck], [eek])
                        if full:
                            ei, eik = EI.next(); qq, qqk = QQ.next(); kk, kkk = KK.next(); am, amk = AM.next()
                            act(ei[:], pc[:, 0:128], AF.Exp, [pck], [eik], scale=-1.0)
                            vstt(qq[:], qg[:, h, :], 128.0 ** -0.5, ee[:], ALU.mult, ALU.mult, [qgk, eek], [qqk])
                            vtt(kk[:], kt[:, h, :], ei[:], ALU.mult, [ktk, eik], [kkk])
                            pa, pak = PS.next()
                            mm(pa[:, 0:128], [(kk[:], qq[:])], [kkk, qqk], [pak])
                            vtt(am[:], pa[:, 0:128], gmk[:, d, :], ALU.mult, [pak, "lgm"], [amk])
                            po = [PS.next(), PS.next()]
                            for vc in range(2):
                                mm(po[vc][0][:, 0:128], [(vg[:, h * 256 + vc * 128:h * 256 + (vc + 1) * 128], am[:])], [vgk, amk], [po[vc][1]], start=True, stop=False)
                        for ci, c in enumerate(co):
                            if full:
                                for vc in range(2):
                                    mm(po[vc][0][:, c * 64:(c + 1) * 64], [(Sb[h][:, vc * 128:(vc + 1) * 128], qq[:, c * 64:(c + 1) * 64])],
                                       [f"lSb{h}", qqk], [po[vc][1]], start=False, stop=(ci == 1))
                            pss, pssk = PS.next()
                            mm(pss[:, 0:256], [(kd[:, c, h * 128:(h + 1) * 128], vg[:, h * 256:(h + 1) * 256])], [kdk, vgk], [pssk])
                            lc = c * 64 + (63 if d == 0 else 0)
                            vstt(Sf[h][:], Sf[h][:], ee[:, lc:lc + 1], pss[:, 0:256], ALU.mult, ALU.add, [f"lSf{h}", eek, pssk], [f"lSf{h}"])
                            act(Sb[h][:], Sf[h][:], AF.Copy, [f"lSf{h}"], [f"lSb{h}"])
                        if full and d == 0:
                            for vc in range(2): evac(ot[:, h * 2 + vc, :], po[vc][0][:, 0:128], [po[vc][1]], [otk])
                        if full and d == 1:
                            osum, osk = OS.next(); sq, sqk = SQ.next(); sil, silk = SIL.next(); rs, rsk = RS.next()
                            for vc in range(2):
                                vtt(osum[:, vc, :], po[vc][0][:, 0:128], oa[:, h * 2 + vc, :], ALU.add, [po[vc][1], oak], [osk])
                            act(sq[:], osum[:], AF.Square, [osk], [sqk])
                            act(sil[:], rt[:, h * 2:h * 2 + 2, :], AF.Silu, [rtk], [silk])
                            p2, p2k = PS.next()
                            mm(p2[:, 0:128], [(onesf[:], sq[:, 0, :]), (onesf[:], sq[:, 1, :])], ["onesf", sqk], [p2k])
                            vts(rs[:], p2[:, 0:128], 1.0 / 256, EPS, ALU.mult, ALU.add, [p2k], [rsk])
                            act(rs[:], rs[:], AF.Sqrt, [rsk], [rsk])
                            vrecip(rs[:], rs[:], [rsk], [rsk])
                            for vc in range(2):
                                tt, ttk = TT.next()
                                vstt(tt[:], osum[:, vc, :], ggc[:, vc:vc + 1], rs[:], ALU.mult, ALU.mult, [osk, "lgg", rsk], [ttk])
                                vtt(go[:, h * 2 + vc, :], tt[:], sil[:, vc, :], ALU.mult, [ttk, silk], [gok])
                    if full and d == 0: store(oAT[:, :, r0:r0 + 128], ot[:], "oAT", ("oAT", tau), [otk])
                    if full and d == 1: store(cTg[:, :, r0:r0 + 128], go[:], "cTg", ("cTg", tau), [gok])
        S.barrier()
        if chk(): return

        with ExitStack() as e1:
            wo = sb("owo", [128, 16, D], BF16, e1)
            wr = sb("owr", [128, KC, NE], F32, e1); brb = sb("obr", [128, NE], F32, e1)
            wrh = sb("owrh", [128, KC, NE], BF16, e1); wrl = sb("owrl", [128, KC, NE], BF16, e1)
            with ExitStack() as e0:
                STG = Pool(e0, nc, "ostg", 2, [128, 16, 128], F32)
                for c in range(16):
                    st, sk = STG.next()
                    load(st[:], w_out[:, c * 128:(c + 1) * 128].rearrange("(h p) n -> p h n", p=128), sk)
                    act(wo[:, :, c * 128:(c + 1) * 128], st[:], AF.Copy, [sk], ["owo"])
                dma("sync", wr[:], w_router.rearrange("(kc p) e -> p kc e", p=128), [], ["owr"], slow=True)
                load(brb[:], b_router.partition_broadcast(128), "obr")
                vcopy(wrh[:], wr[:], ["owr"], ["owrh"])
                vtt(wrl[:], wr[:], wrh[:], ALU.subtract, ["owr", "owrh"], ["owrl"])
            S.barrier()
            XT = Pool(e1, nc, "oxt", 2, [128, D], F32); CT = Pool(e1, nc, "oct", 2, [128, 16, 128], BF16)
            X1 = Pool(e1, nc, "ox1", 1, [128, D], F32); TM = sb("otm", [128, 512], F32, e1); JK = sb("ojk", [128, D], BF16, e1)
            H2 = sb("oh2", [128, D], F32, e1); HBH = sb("ohh", [128, D], BF16, e1); HBL = sb("ohl", [128, D], BF16, e1)
            HBT = Pool(e1, nc, "ohb", 2, [128, KC, 128], BF16); HLT = Pool(e1, nc, "ohlt", 2, [128, KC, 128], BF16)
            LG = sb("olg", [128, NE], F32, e1); EX = sb("oex", [128, NE], F32, e1); MK = sb("omk", [128, NE], F32, e1); T8 = sb("ot8", [128, 8], F32, e1)
            cTa2 = cTa.rearrange("p (c two) t -> p two c t", two=2)
            for i in range(NT if ocut >= 2 else 0):
                xt, xk = XT.next(); ct, ctk = CT.next(); x1, x1k = X1.next()
                load(xt[:], xall[i * 128:(i + 1) * 128, :], xk)
                load(ct[0:64, 0:8, :], cTa2[:, 0, :, i * 128:(i + 1) * 128], ctk, reads=[("cTa", i)])
                load(ct[64:128, 0:8, :], cTa2[:, 1, :, i * 128:(i + 1) * 128], ctk, reads=[("cTa", i)])
                load(ct[:, 8:16, :], cTg[:, :, i * 128:(i + 1) * 128], ctk, reads=[("cTg", i)])
                for n in range(4):
                    cs = slice(n * 512, (n + 1) * 512)
                    p, pk = PS.next()
                    mm(p[:], [(ct[:, c, :], wo[:, c, cs]) for c in range(16)], [ctk, "owo"], [pk])
                    vtt(TM[:], p[:], GTA[:, cs], ALU.mult, [pk, "GTA"], ["otm"])
                    vtt(x1[:, cs], TM[:], xt[:, cs], ALU.add, ["otm", xk], [x1k])
                store(x1s[i * 128:(i + 1) * 128, :], x1[:], "x1s", ("x1s", i), [x1k])
                if ocut < 3: continue
                act(JK[:], x1[:], AF.Square, [x1k], ["ojk", "sm0"], accum_out=sm[:, 0:1])
                r, rk = rstd_from_ss(0, D, "o")
                vstt(H2[:], x1[:], r, A2[:], ALU.mult, ALU.mult, [x1k, rk, "A2"], ["oh2"])
                vtt(H2[:], H2[:], B2[:], ALU.add, ["oh2", "B2"], ["oh2"])
                hbt, hbk = HBT.next(); hlt, hlk = HLT.next()
                vcopy(HBH[:], H2[:], ["oh2"], ["ohh"])
                vtt(HBL[:], H2[:], HBH[:], ALU.subtract, ["oh2", "ohh"], ["ohl"])
                for (src, srck, dstt, dstk, use_act) in [(HBH, "ohh", hbt, hbk, True), (HBL, "ohl", hlt, hlk, False)]:
                    for half in range(2):
                        p, pk = PS.next(); pb = p[:].bitcast(BF16)
                        for j in range(8):
                            kc = half * 8 + j
                            tr(pb[:, j * 128:(j + 1) * 128], src[:, kc * 128:(kc + 1) * 128], identb[:], [srck, "identb"], [pk])
                        pv = pb[:, 0:1024].rearrange("p (a b) -> p a b", a=8)
                        if use_act: act(dstt[:, half * 8:(half + 1) * 8, :], pv, AF.Copy, [pk], [dstk])
                        else: vcopy(dstt[:, half * 8:(half + 1) * 8, :], pv, [pk], [dstk])
                store(h2T[:, :, i * 128:(i + 1) * 128], hbt[:], "h2T", ("h2T", i), [hbk])
                if ocut < 4: continue
                p, pk = PS.next()
                mm(p[:, 0:NE], [(hbt[:, kc, :], wrh[:, kc, :]) for kc in range(KC)] + [(hbt[:, kc, :], wrl[:, kc, :]) for kc in range(KC)]
                   + [(hlt[:, kc, :], wrh[:, kc, :]) for kc in range(KC)], [hbk, hlk, "owrh", "owrl"], [pk])
                vtt(LG[:], p[:, 0:NE], brb[:], ALU.add, [pk, "obr"], ["olg"])
                if ocut < 5: continue
                S.add("vector", lambda e: e.max(out=T8[:], in_=LG[:]), ["olg"], ["ot8"])
                vts(sm[:, 8:9], T8[:, 0:1], -1.0, None, ALU.mult, None, ["ot8"], ["sm8"])
                act(EX[:], LG[:], AF.Exp, ["olg", "sm8"], ["oex"], bias=sm[:, 8:9])
                vts(MK[:], LG[:], T8[:, 3:4], None, ALU.is_ge, None, ["olg", "ot8"], ["omk"])
                vtt(EX[:], EX[:], MK[:], ALU.mult, ["oex", "omk"], ["oex"])
                S.add("vector", lambda e: e.tensor_reduce(out=sm[:, 9:10], in_=EX[:], axis=AX.X, op=ALU.add), ["oex"], ["sm9"])
                vrecip(sm[:, 10:11], sm[:, 9:10], ["sm9"], ["sm10"])
                vts(GATES[:, i, :], EX[:], sm[:, 10:11], None, ALU.mult, None, ["oex", "sm10"], ["GATES"])
        eA.close()
        S.barrier()
        if chk(): return

        w_gu = din("w_gu", [NE, D, 2 * D]); b_gu = din("b_gu", [NE * 32, 128]); w_down = din("w_down", [NE, D, D]); b_down = din("b_down", [NE, D])
        with ExitStack() as e1:
            bguT = sb("mbg", [128, NE * 32], F32, e1); bd = sb("mbd", [NE, D], F32, e1); gT = sb("mgt", [NE, 128], F32, e1)
            XTP = Pool(e1, nc, "mxt", 1, [128, KC, 512], BF16); ACTT = sb("mact", [128, KC, 512], BF16, e1); YACC = sb("myacc", [128, 4, D], F32, e1)
            STG = Pool(e1, nc, "mstg", 2, [128, KC, 256], F32); WGB = Pool(e1, nc, "mwb", 3, [128, KC, 256], BF16)
            GL = Pool(e1, nc, "mgl", 2, [128, 512], F32); SG = Pool(e1, nc, "msg", 2, [128, 512], F32); LN = Pool(e1, nc, "mln", 2, [128, 512], F32)
            X1L = Pool(e1, nc, "mx1", 1, [128, D], F32); YO = Pool(e1, nc, "myo", 1, [128, D], F32); JK = sb("mjk", [128, D], BF16, e1)
            for q in range(NE * 32 // 128):
                st, sk = STG.next()
                load(st[:, 0, 0:128], b_gu[q * 128:(q + 1) * 128, :], sk)
                p, pk = PS.next()
                tr(p[:, 0:128], st[:, 0, 0:128], identf[:], [sk, "identf"], [pk])
                vcopy(bguT[:, q * 128:(q + 1) * 128], p[:, 0:128], [pk], ["mbg"])
            load(bd[:], b_down[:, :], "mbd")
            def wunit(src):
                st, sk = STG.next(); wb, wk = WGB.next()
                load(st[:], src.rearrange("(kc p) c -> p kc c", p=128), sk, eng=("sync" if sk.endswith("0") else "gpsimd"))
                act(wb[:], st[:], AF.Copy, [sk], [wk])
                return wb, wk
            for g in range(TL // 512):
                xt, xk = XTP.next()
                load(xt[:], h2T[:, :, g * 512:(g + 1) * 512], xk, reads=[("h2T", g * 4 + i) for i in range(4)])
                for e in range(NE):
                    for cb in range(8):
                        wg, wgk = wunit(w_gu[e][:, cb * 256:(cb + 1) * 256]); wl, wlk = wunit(w_gu[e][:, D + cb * 256:D + (cb + 1) * 256])
                        for sub in range(2):
                            hc = cb * 2 + sub
                            pg, pgk = PS.next(); pl, plk = PS.next()
                            mm(pg[:], [(wg[:, kc, sub * 128:(sub + 1) * 128], xt[:, kc, :]) for kc in range(KC)], [wgk, xk], [pgk])
                            mm(pl[:], [(wl[:, kc, sub * 128:(sub + 1) * 128], xt[:, kc, :]) for kc in range(KC)], [wlk, xk], [plk])
                            gl, glk = GL.next(); sg, sgk = SG.next(); ln, lnk = LN.next()
                            cg = e * 32 + hc; cl = e * 32 + 16 + hc
                            vts(gl[:], pg[:], bguT[:, cg:cg + 1], 7.0, ALU.add, ALU.min, [pgk, "mbg"], [glk])
                            act(sg[:], gl[:], AF.Sigmoid, [glk], [sgk], scale=1.702)
                            vts(ln[:], pl[:], bguT[:, cl:cl + 1], 7.0, ALU.add, ALU.min, [plk, "mbg"], [lnk])
                            vts(ln[:], ln[:], -7.0, 1.0, ALU.max, ALU.add, [lnk], [lnk])
                            vtt(gl[:], gl[:], sg[:], ALU.mult, [glk, sgk], [glk])
                            vtt(ACTT[:, hc, :], gl[:], ln[:], ALU.mult, [glk, lnk], ["mact"])
                    for nb in range(8):
                        wd, wdk = wunit(w_down[e][:, nb * 256:(nb + 1) * 256])
                        for m in range(4):
                            p, pk = PS.next()
                            mm(p[:, 0:256], [(ACTT[:, hc, m * 128:(m + 1) * 128], wd[:, hc, :]) for hc in range(KC)], ["mact", wdk], [pk])
                            ys = YACC[:, m, nb * 256:(nb + 1) * 256]; gcol = GATES[:, g * 4 + m, e:e + 1]
                            if e == 0: vts(ys, p[:, 0:256], gcol, None, ALU.mult, None, [pk, "GATES"], [("yacc", m)])
                            else: vstt(ys, p[:, 0:256], gcol, ys, ALU.mult, ALU.add, [pk, "GATES", ("yacc", m)], [("yacc", m)])
                for m in range(4):
                    i = g * 4 + m
                    p, pk = PS.next()
                    tr(p[0:NE, 0:128], GATES[:, i, :], identf[:], ["GATES", "identf"], [pk])
                    vcopy(gT[:], p[0:NE, 0:128], [pk], ["mgt"])
                    x1, x1k = X1L.next(); yo, yok = YO.next()
                    load(x1[:], x1s[i * 128:(i + 1) * 128, :], x1k, reads=[("x1s", i)])
                    for n in range(4):
                        cs = slice(n * 512, (n + 1) * 512)
                        p, pk = PS.next()
                        mm(p[:], [(gT[:], bd[:, cs])], ["mgt", "mbd"], [pk])
                        vtt(yo[:, cs], p[:], YACC[:, m, cs], ALU.add, [pk, ("yacc", m)], [yok])
                    vtt(yo[:], yo[:], GTF[:], ALU.mult, [yok, "GTF"], [yok])
                    vtt(yo[:], yo[:], x1[:], ALU.add, [yok, x1k], [yok])
                    act(JK[:], yo[:], AF.Square, [yok], ["mjk", "sm0"], accum_out=sm[:, 0:1])
                    r, rk = rstd_from_ss(0, D, "m")
                    vstt(yo[:], yo[:], r, GFIN[:], ALU.mult, ALU.mult, [yok, rk, "GFIN"], [yok])
                    store(out[i * 128:(i + 1) * 128, :], yo[:], "out", ("out", i), [yok])


    body()
    eA.close()
    nsem = S.plan([("out", i) for i in range(NT)] if stop >= 99 else [])
    print("nsem", nsem, "nops", len(S.ops))
    _burn = [es.enter_context(nc.semaphore(f"burn{i}")) for i in range(burn)]
    sems = [es.enter_context(nc.semaphore(f"sem{i}")) for i in range(nsem)]
    print("sem ids", getattr(sems[0], "num", sems[0]), "..", getattr(sems[-1], "num", sems[-1]))
    block = es.enter_context(nc.Block())
    S.emit(block, sems)
    es.close()
    return nc


def _consts(TL, pos):
    bf = ml_dtypes.bfloat16
    c = {}
    c["c_identf"] = np.eye(128, dtype=np.float32); c["c_identb"] = np.eye(128, dtype=np.float32).astype(bf)
    d = np.arange(128) % 64; f = d % 16; half = (d // 16) % 2; axis = d // 32
    inv = (10000.0 ** (-np.arange(16, dtype=np.float32) / 16)).astype(np.float32)
    row = (pos // GRID_W).astype(np.float32); col = (pos % GRID_W).astype(np.float32)
    p = np.where(axis[:, None] == 0, row[None, :], col[None, :]).astype(np.float32)
    ang = (p * inv[f][:, None]).astype(np.float32)
    c["c_cos"] = np.cos(ang).astype(np.float32)
    c["c_sin"] = (np.sin(ang) * np.where(half == 0, -1.0, 1.0)[:, None]).astype(np.float32)
    i = np.arange(128)[:, None]; j = np.arange(128)[None, :]
    mp = np.where(i >= j, 0.0, NEG).astype(np.float32); mn = np.where(i <= j, 0.0, NEG).astype(np.float32)
    c["c_maskp"] = np.tile(mp, (1, 4)).astype(bf); c["c_maskn"] = np.tile(mn, (1, 4)).astype(bf)
    same = (i // 64) == (j // 64); sc = -1.0 / 16.0
    tri = np.stack([np.where(same & (i <= j), sc, 0.0), np.where(same & (i >= j), sc, 0.0)])
    rev = np.stack([np.where(same & (i > j), sc, 0.0), np.where(same & (i < j), sc, 0.0)])
    gm = np.stack([np.where(same & (i <= j), 1.0, 0.0), np.where(same & (i >= j), 1.0, 0.0)])
    c["c_tri"] = tri.astype(np.float32); c["c_rev"] = rev.astype(np.float32); c["c_gm"] = gm.astype(np.float32)
    return c


def _swap_perm(nheads):
    idx = []
    for h in range(nheads):
        for d in range(64):
            idx.append(h * 64 + (d + 16 if (d // 16) % 2 == 0 else d - 16))
    return np.array(idx)


def make_in_maps(x, c, ctx, c_ctx, w_ada, b_ada, g_mix, g_ffn, w_in, attn_sink, w_gate2, b_gate2, g_gla,
                 w_out, w_router, b_router, w_gu, b_gu, w_down, b_down, g_final):
    f = lambda a: np.ascontiguousarray(np.asarray(a, dtype=np.float32))
    x = f(x); ctx = f(ctx); B, SEQ, _ = x.shape; NE = w_router.shape[-1]; TL = SEQ // 2
    w_in0 = f(w_in[0]); wq = w_in0[:, 0:1024]; wk = w_in0[:, 1024:1280]
    w_qks = np.ascontiguousarray(np.concatenate([wq[:, _swap_perm(16)], wk[:, _swap_perm(4)]], axis=1))
    shared = dict(w_ada=f(w_ada[0]), b_ada=f(b_ada[0]), g_mix=f(g_mix[0]), g_ffn=f(g_ffn[0]), g_final=f(g_final), w_in=w_in0, w_qks=w_qks,
                  attn_sink=f(attn_sink[0]), g_gla=f(g_gla[0]), w_out=f(w_out[0]), w_router=f(w_router[0]), b_router=f(b_router[0]),
                  w_gu=f(w_gu[0]), b_gu=f(b_gu[0]).reshape(NE * 32, 128), w_down=f(w_down[0]), b_down=f(b_down[0]))
    wz = w_in0[:, 4608:4640]
    per_half = []
    for half in range(2):
        pos = np.arange(TL + 128) if half == 0 else (SEQ - 1 - np.arange(TL + 128))
        dd = dict(_consts(TL, pos))
        if half == 0:
            dd["w_z"] = np.ascontiguousarray(wz); dd["w_g2"] = f(w_gate2[0]); dd["b_g2"] = f(b_gate2[0])
        else:
            dd["w_z"] = np.ascontiguousarray(np.concatenate([wz[:, 16:32], wz[:, 0:16]], axis=1))
            dd["w_g2"] = np.ascontiguousarray(f(w_gate2[0])[::-1]); dd["b_g2"] = np.ascontiguousarray(f(b_gate2[0])[::-1])
        per_half.append(dd)
    in_maps = []
    for b in range(B):
        for half in range(2):
            m = dict(shared); m.update(per_half[half])
            m["xall"] = x[b] if half == 0 else np.ascontiguousarray(x[b][::-1])
            m["ctxl"] = ctx[b] if half == 0 else np.ascontiguousarray(ctx[b][::-1])
            m["cvec"] = np.ascontiguousarray(np.stack([f(c)[b], f(c_ctx)]))
            in_maps.append(m)
    return in_maps, B, SEQ, NE


_NC_CACHE = {}


def kernel(**inputs):
    in_maps, B, SEQ, NE = make_in_maps(**inputs)
    key = (SEQ, NE)
    if key not in _NC_CACHE: _NC_CACHE[key] = build(SEQ, NE)
    res = run_bass_kernel_spmd(_NC_CACHE[key], in_maps, core_ids=list(range(2 * B)))
    TL = SEQ // 2
    out = np.empty((B, SEQ, D), np.float32)
    for b in range(B):
        out[b, 0:TL] = res.results[2 * b]["out"]
        out[b, TL:SEQ] = res.results[2 * b + 1]["out"][::-1]
    return out
```
